# Optimizing a Trainium2 kernel written in Bass

```python
import math
import jax
import jax.numpy as jnp
from jax import lax
import numpy as np

D_MODEL = 1024
BATCH = 1
SEQ = 16384
DEPTH = 2

GRID_W = 64
CTX_LEN = 256
BRANCH_W = 512
NA_HEADS = 8
NA_DH = BRANCH_W // NA_HEADS
NA_WIN_R = 8
NA_WIN_C = 16
DA_HEADS = 4
DA_DH = 64
DA_VD = 2 * DA_DH
DA_QK = DA_HEADS * 2 * DA_DH
GM_GROUPS = 4
GM_CHUNK = 128
GM_GW = BRANCH_W // GM_GROUPS
N_BRANCH = 3
N_EXPERTS = 16
EC_CAPACITY = 2
EXPERT_FF = 1024
Q_BLOCK = 128
ROPE_THETA = 10000.0
ROPE_AXIS = DA_DH // 2
N_MOD = 6
EPS = 1e-6
KV_SIZES = (BRANCH_W, BRANCH_W, DA_QK, DA_HEADS * DA_VD)
Q_SIZES = (BRANCH_W, DA_QK, BRANCH_W, BRANCH_W, N_BRANCH * D_MODEL)
KV_COLS = sum(KV_SIZES)
PROJ_COLS = KV_COLS + sum(Q_SIZES)

kernel_name = "hybrid_na_diff_gmlp_ec_block"


def _split(z, sizes):
    offs = np.cumsum(sizes)[:-1].tolist()
    return jnp.split(z, offs, axis=-1)


def rmsnorm(x, g):
    xf = x.astype(jnp.float32)
    y = xf * lax.rsqrt(jnp.mean(xf * xf, axis=-1, keepdims=True) + EPS)
    return y.astype(x.dtype) * g


def layernorm(x, g, b):
    xf = x.astype(jnp.float32)
    mu = jnp.mean(xf, axis=-1, keepdims=True)
    var = jnp.mean(jnp.square(xf - mu), axis=-1, keepdims=True)
    return ((xf - mu) * lax.rsqrt(var + EPS)).astype(x.dtype) * g + b


def modulate(h, shift, scale):
    return h * (1 + scale) + shift


def rope_tables(n):
    t = jnp.arange(n)
    inv = ROPE_THETA ** (-jnp.arange(0, ROPE_AXIS, 2, dtype=jnp.float32) / ROPE_AXIS)
    ang_r = (t // GRID_W).astype(jnp.float32)[:, None] * inv
    ang_c = (t % GRID_W).astype(jnp.float32)[:, None] * inv
    return (jnp.cos(ang_r), jnp.sin(ang_r), jnp.cos(ang_c), jnp.sin(ang_c))


def _rot(x, cos, sin):
    x1, x2 = jnp.split(x, 2, axis=-1)
    return jnp.concatenate([x1 * cos - x2 * sin, x1 * sin + x2 * cos], axis=-1)


def rope_2d(x, tabs):
    cr, sr, cc, sc = [t.astype(x.dtype)[None, :, None, None, :] for t in tabs]
    xr, xcol = jnp.split(x, 2, axis=-1)
    return jnp.concatenate([_rot(xr, cr, sr), _rot(xcol, cc, sc)], axis=-1)


def dense_attn(q, k, v):
    B, n, H, dh = q.shape
    s = jnp.einsum('bqhd,bkhd->bhqk', q, k) * (dh ** -0.5)
    p = jax.nn.softmax(s.astype(jnp.float32), axis=-1).astype(v.dtype)
    return jnp.einsum('bhqk,bkhd->bqhd', p, v).reshape(B, n, H * dh)


def neighbourhood_attn(q, k, v, kc, vc, rpb):
    B, L, H, dh = q.shape
    Lc = kc.shape[1]
    rows = L // GRID_W
    wr = min(NA_WIN_R, rows)
    scale = dh ** -0.5
    kg = k.reshape(B, rows, GRID_W, H, dh)
    vg = v.reshape(B, rows, GRID_W, H, dh)
    qg = jnp.moveaxis(q.reshape(B, rows, GRID_W, H, dh), 1, 0)
    col = jnp.arange(GRID_W)
    col_start = jnp.clip(col - NA_WIN_C // 2, 0, GRID_W - NA_WIN_C)
    col_idx = col_start[:, None] + jnp.arange(NA_WIN_C)[None, :]
    dc = col_idx - col[:, None] + (NA_WIN_C - 1)

    def row_block(args):
        r, qr = args
        r0 = jnp.clip(r - wr // 2, 0, rows - wr)
        kw = lax.dynamic_slice_in_dim(kg, r0, wr, axis=1)[:, :, col_idx]
        vw = lax.dynamic_slice_in_dim(vg, r0, wr, axis=1)[:, :, col_idx]
        dr = r0 + jnp.arange(wr) - r + (NA_WIN_R - 1)
        bias = jnp.transpose(rpb[:, dr[:, None, None], dc[None]], (0, 2, 1, 3))
        s_loc = jnp.einsum('bqhd,brqjhd->bhqrj', qr, kw) * scale + bias[None]
        s_ctx = jnp.einsum('bqhd,bchd->bhqc', qr, kc) * scale
        s = jnp.concatenate([s_ctx, s_loc.reshape(B, H, GRID_W, wr * NA_WIN_C)], axis=-1)
        p = jax.nn.softmax(s.astype(jnp.float32), axis=-1).astype(v.dtype)
        p_loc = p[..., Lc:].reshape(B, H, GRID_W, wr, NA_WIN_C)
        return (jnp.einsum('bhqc,bchd->bqhd', p[..., :Lc], vc)
                + jnp.einsum('bhqrj,brqjhd->bqhd', p_loc, vw))

    out = lax.map(row_block, (jnp.arange(rows), qg))
    return jnp.moveaxis(out, 0, 1).reshape(B, L, H * dh)


def diff_attend(q, k, v, lam):
    s = jnp.einsum('bqhcd,bkhcd->bhcqk', q, k) * (q.shape[-1] ** -0.5)
    p = jax.nn.softmax(s.astype(jnp.float32), axis=-1)
    a = (p[:, :, 0] - lam * p[:, :, 1]).astype(v.dtype)
    return jnp.einsum('bhqk,bkhd->bqhd', a, v)


def diff_attn_blocks(q, k, v, lam):
    B, L, H, _, dh = q.shape
    nb = L // Q_BLOCK
    qblk = jnp.moveaxis(q.reshape(B, nb, Q_BLOCK, H, 2, dh), 1, 0)
    out = lax.map(lambda qb: diff_attend(qb, k, v, lam), qblk)
    return jnp.moveaxis(out, 0, 1).reshape(B, L, H, v.shape[-1])


def diff_head_norm(o, g, lam_init):
    B, n = o.shape[:2]
    return (rmsnorm(o, g) * (1.0 - lam_init)).reshape(B, n, -1)


def gmlp_gate(u, v, ln_g, ln_b, w_s, b_s):
    B, n, _ = u.shape
    vn = layernorm(v, ln_g, ln_b).reshape(B, n // GM_CHUNK, GM_CHUNK, GM_GROUPS, GM_GW)
    mixed = jnp.einsum('gpq,bnqgc->bnpgc', w_s, vn) + jnp.swapaxes(b_s, 0, 1)[:, :, None]
    return u * mixed.reshape(B, n, BRANCH_W)


def branch_merge(ya, yb, yc, gates, w_branch, w_out):
    ga, gb, gc = jnp.split(jax.nn.sigmoid(gates), N_BRANCH, axis=-1)
    m = ga * (ya @ w_branch[0]) + gb * (yb @ w_branch[1]) + gc * (yc @ w_branch[2])
    return m @ w_out


def ec_moe(h, w_r, w1, w3, w2):
    B, n, D = h.shape
    cap = max(1, EC_CAPACITY * n // N_EXPERTS)
    aff = jax.nn.softmax((h @ w_r).astype(jnp.float32), axis=-1)
    gate, idx = lax.top_k(jnp.swapaxes(aff, 1, 2), cap)
    xin = jax.vmap(lambda hb, ib: hb[ib])(h, idx)
    hid = jax.nn.silu(jnp.einsum('becd,edf->becf', xin, w1)) * jnp.einsum('becd,edf->becf', xin, w3)
    ye = jnp.einsum('becf,efd->becd', hid, w2) * gate[..., None].astype(h.dtype)
    return jax.vmap(lambda yb, ib: jnp.zeros((n, D), h.dtype).at[ib.reshape(-1)].add(yb.reshape(-1, D)))(ye, idx)


def _na_heads(t):
    return t.reshape(t.shape[0], t.shape[1], NA_HEADS, NA_DH)


def _da_qk(t):
    return t.reshape(t.shape[0], t.shape[1], DA_HEADS, 2, DA_DH)


def _da_v(t):
    return t.reshape(t.shape[0], t.shape[1], DA_HEADS, DA_VD)


def setup_inputs(seed: int = 0) -> dict:
    key = jax.random.key(seed)
    ks = jax.random.split(key, 32)
    f32 = jnp.float32

    def nrm(k, shape, scale):
        return jax.random.normal(k, shape, f32) * scale

    D = D_MODEL
    return {
        "x": nrm(ks[0], (BATCH, SEQ, D), 1.0),
        "c": nrm(ks[1], (BATCH, D), 1.0),
        "ctx": nrm(ks[2], (BATCH, CTX_LEN, D), 1.0),
        "c_ctx": nrm(ks[3], (D,), 1.0),
        "w_ada": nrm(ks[4], (DEPTH, D, N_MOD * D), 0.5 * D ** -0.5),
        "b_ada": nrm(ks[5], (DEPTH, N_MOD * D), 0.02),
        "g_norm1": 1.0 + nrm(ks[6], (DEPTH, D), 0.05),
        "g_norm2": 1.0 + nrm(ks[7], (DEPTH, D), 0.05),
        "w_in": nrm(ks[8], (DEPTH, D, PROJ_COLS), D ** -0.5),
        "na_rpb": nrm(ks[9], (DEPTH, NA_HEADS, 2 * NA_WIN_R - 1, 2 * NA_WIN_C - 1), 0.1),
        "da_lam_q1": nrm(ks[10], (DEPTH, DA_DH), 0.1),
        "da_lam_k1": nrm(ks[11], (DEPTH, DA_DH), 0.1),
        "da_lam_q2": nrm(ks[12], (DEPTH, DA_DH), 0.1),
        "da_lam_k2": nrm(ks[13], (DEPTH, DA_DH), 0.1),
        "da_subln_g": 1.0 + nrm(ks[14], (DEPTH, DA_VD), 0.05),
        "gm_ln_g": 1.0 + nrm(ks[15], (DEPTH, BRANCH_W), 0.05),
        "gm_ln_b": nrm(ks[16], (DEPTH, BRANCH_W), 0.02),
        "gm_w_s": nrm(ks[17], (DEPTH, GM_GROUPS, GM_CHUNK, GM_CHUNK), GM_CHUNK ** -0.5),
        "gm_b_s": 1.0 + nrm(ks[18], (DEPTH, GM_GROUPS, GM_CHUNK), 0.1),
        "w_branch": nrm(ks[19], (DEPTH, N_BRANCH, BRANCH_W, D), BRANCH_W ** -0.5),
        "w_out": nrm(ks[20], (DEPTH, D, D), D ** -0.5),
        "w_router": nrm(ks[21], (DEPTH, D, N_EXPERTS), D ** -0.5),
        "w_e1": nrm(ks[22], (DEPTH, N_EXPERTS, D, EXPERT_FF), D ** -0.5),
        "w_e3": nrm(ks[23], (DEPTH, N_EXPERTS, D, EXPERT_FF), D ** -0.5),
        "w_e2": nrm(ks[24], (DEPTH, N_EXPERTS, EXPERT_FF, D), EXPERT_FF ** -0.5),
        "g_final": 1.0 + nrm(ks[25], (D,), 0.05),
    }


def reference(x, c, ctx, c_ctx, w_ada, b_ada, g_norm1, g_norm2, w_in, na_rpb,
              da_lam_q1, da_lam_k1, da_lam_q2, da_lam_k2, da_subln_g,
              gm_ln_g, gm_ln_b, gm_w_s, gm_b_s, w_branch, w_out,
              w_router, w_e1, w_e3, w_e2, g_final):
    L = x.shape[1]
    tabs = rope_tables(L)
    xc = ctx
    for i in range(DEPTH):
        last = i == DEPTH - 1
        mod = jax.nn.silu(c) @ w_ada[i] + b_ada[i]
        sh1, sc1, gt1, sh2, sc2, gt2 = jnp.split(mod[:, None, :], N_MOD, axis=-1)
        modc = jax.nn.silu(c_ctx) @ w_ada[i] + b_ada[i]
        sh1c, sc1c, gt1c, sh2c, sc2c, gt2c = jnp.split(modc, N_MOD, axis=-1)
        lam_init = 0.8 - 0.6 * math.exp(-0.3 * i)
        lam = (jnp.exp(jnp.sum(da_lam_q1[i].astype(jnp.float32) * da_lam_k1[i].astype(jnp.float32)))
               - jnp.exp(jnp.sum(da_lam_q2[i].astype(jnp.float32) * da_lam_k2[i].astype(jnp.float32)))
               + lam_init)

        h = modulate(rmsnorm(x, g_norm1[i]), sh1, sc1)
        hc = modulate(rmsnorm(xc, g_norm1[i]), sh1c, sc1c)
        ka, va, kb, vb, qa, qb, u, v, gates = _split(h @ w_in[i], KV_SIZES + Q_SIZES)
        if last:
            kac, vac, kbc, vbc = _split(hc @ w_in[i][:, :KV_COLS], KV_SIZES)
        else:
            kac, vac, kbc, vbc, qac, qbc, uc, vcc, gatesc = _split(hc @ w_in[i], KV_SIZES + Q_SIZES)

        ya = neighbourhood_attn(_na_heads(qa), _na_heads(ka), _na_heads(va),
                                _na_heads(kac), _na_heads(vac), na_rpb[i])
        kb_all = jnp.concatenate([_da_qk(kbc), rope_2d(_da_qk(kb), tabs)], axis=1)
        vb_all = jnp.concatenate([_da_v(vbc), _da_v(vb)], axis=1)
        yb = diff_head_norm(diff_attn_blocks(rope_2d(_da_qk(qb), tabs), kb_all, vb_all, lam),
                            da_subln_g[i], lam_init)
        yc = gmlp_gate(jax.nn.gelu(u), jax.nn.gelu(v), gm_ln_g[i], gm_ln_b[i], gm_w_s[i], gm_b_s[i])
        x = x + gt1 * branch_merge(ya, yb, yc, gates, w_branch[i], w_out[i])
        if not last:
            yac = dense_attn(_na_heads(qac), _na_heads(kac), _na_heads(vac))
            ybc = diff_head_norm(diff_attend(_da_qk(qbc), _da_qk(kbc), _da_v(vbc), lam),
                                 da_subln_g[i], lam_init)
            ycc = gmlp_gate(jax.nn.gelu(uc), jax.nn.gelu(vcc), gm_ln_g[i], gm_ln_b[i], gm_w_s[i], gm_b_s[i])
            xc = xc + gt1c * branch_merge(yac, ybc, ycc, gatesc, w_branch[i], w_out[i])

        h2 = modulate(rmsnorm(x, g_norm2[i]), sh2, sc2)
        x = x + gt2 * ec_moe(h2, w_router[i], w_e1[i], w_e3[i], w_e2[i])
        if not last:
            h2c = modulate(rmsnorm(xc, g_norm2[i]), sh2c, sc2c)
            xc = xc + gt2c * ec_moe(h2c, w_router[i], w_e1[i], w_e3[i], w_e2[i])
    return rmsnorm(x, g_final)
```

```python
import math
from contextlib import ExitStack
import numpy as np
import concourse.bass as bass
import concourse.mybir as mybir
from concourse.bass_utils import run_bass_kernel_spmd

F32 = mybir.dt.float32
BF16 = mybir.dt.bfloat16
U32 = mybir.dt.uint32
AF = mybir.ActivationFunctionType
ALU = mybir.AluOpType
AX = mybir.AxisListType

NCORES = 8
D = 1024
L = 16384
TPC = L // NCORES
NLT = TPC // 128
CTX = 256
EPS = 1e-6
NKT = (L + CTX) // 128
NHALO = 20
NEG = -30000.0


class Res:
    __slots__ = ("w", "r", "excl")

    def __init__(self):
        self.w = None
        self.r = []
        self.excl = False


class Op:
    __slots__ = ("eng", "fn", "deps", "signal", "val", "is_dma", "sem")

    def __init__(self, eng, fn, is_dma=False):
        self.eng = eng
        self.fn = fn
        self.deps = []
        self.signal = False
        self.val = None
        self.is_dma = is_dma
        self.sem = None


ENGS = ("pe", "act", "dve", "pool", "sp")
ENGOBJ = {"pe": "tensor", "act": "scalar", "dve": "vector", "pool": "gpsimd", "sp": "sync"}


class KB:
    def __init__(self, nc, n_dma_sems=16):
        self.nc = nc
        self.ops = {e: [] for e in ENGS}
        self.eng_sem = {e: nc.alloc_semaphore("sem_" + e) for e in ENGS}
        self.dma_sems = {q: [nc.alloc_semaphore("dsem_%s_%d" % (q, i)) for i in range(n_dma_sems)]
                         for q in ("sp", "pool")}
        self.dma_rr = {q: 0 for q in ("sp", "pool")}
        self.dma_last = {}
        self.dma_cnt = {}
        self.all_ops = []
        self.all_res = []
        self.hist = {e: None for e in ENGS}
        self.cnt = {e: 0 for e in ENGS}
        self.known = {e: {} for e in ENGS}
        self.nops = 0

    def res(self):
        r = Res()
        self.all_res.append(r)
        return r

    def _track(self, op, r, w):
        deps = op.deps
        if any(x.excl for x in r):
            w = list(w) + [x for x in r if x.excl and x not in w]
            r = [x for x in r if not x.excl]
        for x in r:
            if x.w is not None:
                deps.append(x.w)
        for x in w:
            if x.w is not None:
                deps.append(x.w)
            deps.extend(x.r)
        for x in r:
            x.r.append(op)
        for x in w:
            x.w = op
            x.r = []

    def op(self, eng, fn, r=(), w=()):
        o = Op(eng, fn)
        self._track(o, r, w)
        self.ops[eng].append(o)
        self.all_ops.append(o)
        self.nops += 1
        return o

    def dma(self, q, fn, r=(), w=()):
        o = Op(q, fn, is_dma=True)
        sems = self.dma_sems[q]
        s = sems[self.dma_rr[q] % len(sems)]
        self.dma_rr[q] += 1
        prev = self.dma_last.get(id(s))
        if prev is not None:
            o.deps.append(prev)
        self.dma_last[id(s)] = o
        self.dma_cnt[id(s)] = self.dma_cnt.get(id(s), 0) + 1
        o.sem = s
        o.val = 16 * self.dma_cnt[id(s)]
        o.signal = True
        self._track(o, r, w)
        self.ops[q].append(o)
        self.all_ops.append(o)
        self.nops += 1
        return o

    def barrier(self):
        lasts = []
        for e in ENGS:
            last = self.hist[e]
            for o in self.ops[e]:
                if not o.is_dma:
                    last = o
            if last is not None:
                lasts.append(last)
        lasts += list(self.dma_last.values())
        for e in ENGS:
            o = Op(e, lambda eng: eng.nop())
            o.deps = list(lasts)
            self.ops[e].append(o)
            self.all_ops.append(o)

    def flush(self, final_wait_ops=()):
        nc = self.nc
        for o in self.all_ops:
            for d in o.deps:
                if not d.is_dma:
                    if d.eng == "pe" and o.eng == "pe" and not o.is_dma:
                        continue
                    d.signal = True
        for rs in self.all_res:
            if rs.w is not None:
                rs.w.signal = True
            for o in rs.r:
                o.signal = True
        for e in ENGS:
            c = self.cnt[e]
            lastc = None
            for o in self.ops[e]:
                if not o.is_dma:
                    lastc = o
            if lastc is not None:
                lastc.signal = True
            for o in self.ops[e]:
                if o.is_dma:
                    continue
                if o.signal:
                    c += 1
                    o.val = c
                    o.sem = self.eng_sem[e]
            self.cnt[e] = c
        with nc.Block() as block:
            for e in ENGS:
                ops = self.ops[e]
                if not ops and not (e == "sp" and final_wait_ops):
                    continue

                def body(eng, ops=ops, e=e):
                    known = self.known[e]
                    for o in ops:
                        need = {}
                        for d in o.deps:
                            if (not d.is_dma) and d.eng == "pe" and e == "pe" and not o.is_dma:
                                continue
                            k = id(d.sem)
                            if known.get(k, 0) >= d.val:
                                continue
                            if k not in need or need[k][1] < d.val:
                                need[k] = (d.sem, d.val)
                        for k, (s, v) in need.items():
                            eng.wait_ge(s, v)
                            known[k] = v
                        ins = o.fn(eng)
                        if o.signal:
                            ins.then_inc(o.sem, 16 if o.is_dma else 1)
                    if e == "sp":
                        for o in final_wait_ops:
                            if known.get(id(o.sem), 0) < o.val:
                                eng.wait_ge(o.sem, o.val)
                                known[id(o.sem)] = o.val

                getattr(block, ENGOBJ[e])(body)
        for e in ENGS:
            for o in self.ops[e]:
                o.fn = None
                if not o.is_dma:
                    self.hist[e] = o
            self.ops[e] = []
        self.all_ops = []


class Ring:
    def __init__(self, items):
        self.items = items
        self.i = 0

    def next(self):
        it = self.items[self.i % len(self.items)]
        self.i += 1
        return it


class Builder:
    def __init__(self, name):
        self.nc = bass.Bass("TRN2", target_bir_lowering=False)
        self.kb = KB(self.nc)
        self.stack = []
        self.outs = []
        nc = self.nc
        self.banks = [nc.alloc_psum_tensor("bank%d" % i, [128, 512], F32) for i in range(8)]
        self.bres = [self.kb.res() for _ in range(8)]
        for rr in self.bres:
            rr.excl = True
        self.uid = 0

    def scope(self):
        st = ExitStack()
        self.stack.append(st)
        return st

    def end_scope(self):
        self.kb.barrier()
        self.kb.flush()
        self.stack.pop().close()

    def T(self, name, shape, dt):
        self.uid += 1
        return self.stack[-1].enter_context(self.nc.sbuf_tensor("%s_%d" % (name, self.uid), list(shape), dt))

    def TR(self, name, shape, dt):
        return self.T(name, shape, dt), self.kb.res()

    def ring(self, name, n, shape, dt):
        return Ring([self.TR(name + str(i), shape, dt) for i in range(n)])

    def bank_ring(self, idxs):
        return Ring([(self.banks[i], self.bres[i]) for i in idxs])

    def din(self, name, shape, dt=F32):
        return self.nc.dram_tensor(name, list(shape), dt, kind="ExternalInput").ap()

    def dout(self, name, shape, dt=F32):
        return self.nc.dram_tensor(name, list(shape), dt, kind="ExternalOutput").ap()

    def dscr(self, name, shape, dt):
        return self.nc.dram_tensor(name, list(shape), dt, kind="Internal").ap()

    def mm(self, out, lhsT, rhs, start, stop, r, w):
        return self.kb.op("pe", lambda e: e.matmul(out, lhsT=lhsT, rhs=rhs, start=start, stop=stop), r=r, w=w)

    def tr(self, out, in_, ident, r, w):
        return self.kb.op("pe", lambda e: e.transpose(out=out, in_=in_, identity=ident), r=r, w=w)

    def act(self, out, in_, func, r, w, **kw):
        return self.kb.op("act", lambda e: e.activation(out=out, in_=in_, func=func, **kw), r=r, w=w)

    def tt(self, out, in0, in1, op, r, w, eng="dve"):
        return self.kb.op(eng, lambda e: e.tensor_tensor(out=out, in0=in0, in1=in1, op=op), r=r, w=w)

    def ts(self, out, in0, s1, s2, op0, op1, r, w, eng="dve"):
        if op1 is None:
            return self.kb.op(eng, lambda e: e.tensor_scalar(out=out, in0=in0, scalar1=s1, scalar2=None, op0=op0), r=r, w=w)
        return self.kb.op(eng, lambda e: e.tensor_scalar(out=out, in0=in0, scalar1=s1, scalar2=s2, op0=op0, op1=op1), r=r, w=w)

    def stt(self, out, in0, scalar, in1, op0, op1, r, w):
        return self.kb.op("dve", lambda e: e.scalar_tensor_tensor(out=out, in0=in0, scalar=scalar, in1=in1, op0=op0, op1=op1), r=r, w=w)

    def copy(self, out, in_, r, w, eng="dve"):
        if eng == "act":
            return self.kb.op("act", lambda e: e.copy(out=out, in_=in_), r=r, w=w)
        return self.kb.op(eng, lambda e: e.tensor_copy(out, in_), r=r, w=w)

    def memset(self, ap, val, w, eng="dve"):
        return self.kb.op(eng, lambda e: e.memset(ap, val), w=w)

    def ld(self, out, in_, w, r=(), q="sp"):
        return self.kb.dma(q, lambda e: e.dma_start(out=out, in_=in_), r=r, w=w)

    def st(self, out, in_, r, w=(), q="sp"):
        o = self.kb.dma(q, lambda e: e.dma_start(out=out, in_=in_), r=r, w=w)
        return o

    def consts(self):
        kb = self.kb
        self.identf, self.r_id = self.TR("identf", [128, 128], F32)
        self.identb = self.T("identb", [128, 128], BF16)
        self.mhalf = self.T("mhalf", [128, 4], F32)
        kb.op("pool", lambda e: e.memset(self.identf[:], 0.0), w=[self.r_id])
        kb.op("pool", lambda e: e.affine_select(out=self.identf[:], in_=self.identf[:], pattern=[[-1, 128]],
                                                 compare_op=ALU.not_equal, fill=1.0, base=0, channel_multiplier=1),
              r=[self.r_id], w=[self.r_id])
        self.copy(self.identb[:], self.identf[:], r=[self.r_id], w=[self.r_id])
        kb.op("pool", lambda e: e.memset(self.mhalf[:], -0.5), w=[self.r_id])

    def load_w_bf16(self, name, src_ap, kchunks, ncols):
        t, r = self.TR(name, [128, kchunks, ncols], BF16)
        src = src_ap.rearrange("(k p) n -> p k n", p=128)
        for c0 in range(0, ncols, 1024):
            c1 = min(ncols, c0 + 1024)
            step = max(1, 4096 // (c1 - c0))
            for k0 in range(0, kchunks, step):
                k1 = min(kchunks, k0 + step)
                self.ld(t[:, k0:k1, c0:c1], src[:, k0:k1, c0:c1], w=[r], q="pool")
        return t, r

    def bcast(self, name, row_ap, n):
        t, r = self.TR(name, [128, n], F32)
        self.ld(t[:], row_ap.partition_broadcast(128), w=[r])
        return t, r

    def norm_setup(self):
        self.r_junk = self.ring("junk", 2, [128, 1024], F32)
        self.r_stat = self.ring("nstat", 3, [128, 4], F32)
        self.r_ntmp = self.ring("ntmp", 2, [128, 1024], F32)

    def rstd_of(self, x_ap, n, r_x):
        junk, rj = self.r_junk.next()
        stt, rs = self.r_stat.next()
        self.act(junk[:, 0:n], x_ap, AF.Square, r=[r_x], w=[rj, rs], accum_out=stt[:, 0:1])
        self.ts(stt[:, 1:2], stt[:, 0:1], 1.0 / n, EPS, ALU.mult, ALU.add, r=[rs], w=[rs])
        self.kb.op("pool", lambda e: e.tensor_tensor(out=stt[:, 2:3], in0=stt[:, 1:2], in1=self.mhalf[:, 0:1], op=ALU.pow),
                   r=[rs, self.r_id], w=[rs])
        return stt[:, 2:3], rs

    def norm_mod(self, x_ap, r_x, A, rA, sh, rsh, out_ap, r_out):
        rstd, rs = self.rstd_of(x_ap, 1024, r_x)
        tmp, rt = self.r_ntmp.next()
        self.stt(tmp[:], x_ap, rstd, A[:], ALU.mult, ALU.mult, r=[r_x, rs, rA], w=[rt])
        self.tt(out_ap, tmp[:], sh[:], ALU.add, r=[rt, rsh], w=[r_out])

    def transpose_to(self, src_tile, r_src, nchunk, dst_ap_fn, r_dst, bank, rb, evac="dve"):
        pb = bank.bitcast(BF16)
        for k in range(nchunk):
            self.tr(pb[:, k * 128:(k + 1) * 128], src_tile[:, k * 128:(k + 1) * 128], self.identb[:], r=[r_src, self.r_id], w=[rb])
        self.copy(dst_ap_fn, pb[:, 0:nchunk * 128].rearrange("p (k t) -> p k t", k=nchunk), r=[rb], w=[r_dst], eng=evac)


DBG = False
STOP = None


def build_mix(last, lam_init, dbg=None):
    dbg = DBG if dbg is None else dbg
    B = Builder("mix")
    nc, kb = B.nc, B.kb
    NQ = NLT + (0 if last else 2)
    NQT = NQ * 128
    NHC = NHALO + 2

    xfull = B.din("xfull", [L, D])
    ctx = B.din("ctx", [CTX, D])
    xhalo = B.din("xhalo", [NHALO * 128, D])
    cvec = B.din("cvec", [16, 128])
    w_ada = B.din("w_ada", [D, 6 * D])
    b_ada = B.din("b_ada", [1, 6 * D])
    g1 = B.din("g1", [1, D])
    g2 = B.din("g2", [1, D])
    w_in = B.din("w_in", [D, 7168])
    w_sw = B.din("w_sw", [D, 1024])
    lamv = B.din("lamv", [1, 256])
    subg = B.din("subg", [1, 128])
    ln_g = B.din("ln_g", [1, 512])
    ln_b = B.din("ln_b", [1, 512])
    wsT = B.din("wsT", [4, 128, 128])
    bsT = B.din("bsT", [128, 4])
    w_br = B.din("w_br", [1536, D])
    w_out = B.din("w_out", [D, D])
    w_r = B.din("w_r", [D, 16])
    ropeF = B.din("ropeF", [2, 128, L + CTX])
    ropeQ = B.din("ropeQ", [2, 128, TPC + CTX])
    nabias = B.din("nabias", [NLT, 8, 128, 768])

    xmid = B.dout("xmid", [NQT, D])
    aff = B.dout("aff", [NQT, 16])
    modout = B.dout("modout", [2, 6 * D])
    modscr = B.dscr("modscr", [2, 6 * D], F32)
    KbT = B.dscr("KbT", [4, 128, L + CTX], BF16)
    Vb = B.dscr("Vb", [4, 128, NKT, 129], BF16)
    if dbg:
        yT = B.dout("yT", [3, 128, 4, NQT], BF16)
    else:
        yT = B.dscr("yT", [3, 128, 4, NQT], BF16)
    r_modscr = kb.res(); r_KbT = kb.res(); r_Vb = kb.res(); r_yT = [kb.res() for _ in range(3)]
    fin = []

    def finish():
        kb.barrier()
        kb.flush(final_wait_ops=fin)
        while B.stack:
            B.stack.pop().close()
        return nc

    B.scope()
    B.consts()
    B.norm_setup()
    hT_hc, r_hT = B.TR("hT_hc", [128, 8, NHC * 128], BF16)
    neglam, r_lam = B.TR("neglam", [128, 4], F32)
    br_tp = B.bank_ring([6, 7])
    br_a = B.bank_ring([0, 1, 2])
    br_b = B.bank_ring([3, 4, 5])

    B.scope()
    cs, r_cs = B.TR("cs", [128, 128], F32)
    B.memset(cs[:], 0.0, w=[r_cs])
    B.ld(cs[0:16, :], cvec, w=[r_cs])
    bk, rb = br_a.next()
    B.tr(bk[:, 0:128], cs[:], B.identf[:], r=[r_cs, B.r_id], w=[rb])
    th, r_th = B.TR("th", [128, 16], F32)
    csT, r_csT = B.TR("csT", [128, 16], F32)
    B.act(th[:], bk[:, 0:16], AF.Tanh, r=[rb], w=[r_th], scale=0.5)
    B.stt(csT[:], th[:], 1.0, bk[:, 0:16], ALU.add, ALU.mult, r=[r_th, rb], w=[r_csT])
    B.ts(csT[:], csT[:], 0.5, None, ALU.mult, None, r=[r_csT], w=[r_csT])
    csbig, r_csbig = B.TR("csbig", [128, 8, 128], F32)
    B.memset(csbig[:], 0.0, w=[r_csbig])
    B.copy(csbig[:, :, 0:2], csT[:, 0:16].rearrange("p (k a) -> p k a", a=2), r=[r_csT, r_csbig], w=[r_csbig])
    bada, r_bada = B.TR("bada", [2, 6 * D], F32)
    B.ld(bada[:], b_ada.partition_broadcast(2), w=[r_bada])
    modsb, r_modsb = B.TR("modsb", [2, 6 * D], F32)
    wa_ring = B.ring("wada", 2, [128, 8, 512], F32)
    for nb in range(12):
        wa, rwa = wa_ring.next()
        B.ld(wa[:], w_ada[:, nb * 512:(nb + 1) * 512].rearrange("(k p) n -> p k n", p=128), w=[rwa])
        bk, rb = br_a.next()
        for k in range(8):
            B.mm(bk[:, :], csbig[:, k, :], wa[:, k, :], k == 0, k == 7, r=[r_csbig, rwa], w=[rb])
        B.tt(modsb[:, nb * 512:(nb + 1) * 512], bk[0:2, :], bada[:, nb * 512:(nb + 1) * 512], ALU.add, r=[rb, r_bada], w=[r_modsb])
    B.st(modscr, modsb[:], r=[r_modsb], w=[r_modscr])
    fin.append(B.st(modout, modsb[:], r=[r_modsb]))
    lv, r_lv = B.bcast("lv", lamv, 256)
    lp, r_lp = B.TR("lp", [128, 128], F32)
    ls, r_ls = B.TR("ls", [128, 4], F32)
    B.tt(lp[:, 0:64], lv[:, 0:64], lv[:, 64:128], ALU.mult, r=[r_lv], w=[r_lp])
    B.tt(lp[:, 64:128], lv[:, 128:192], lv[:, 192:256], ALU.mult, r=[r_lv], w=[r_lp])
    kb.op("dve", lambda e: e.tensor_reduce(out=ls[:, 0:2], in_=lp[:].rearrange("p (a d) -> p a d", a=2), axis=AX.X, op=ALU.add), r=[r_lp], w=[r_ls])
    B.act(ls[:, 2:4], ls[:, 0:2], AF.Exp, r=[r_ls], w=[r_ls])
    B.tt(neglam[:, 0:1], ls[:, 3:4], ls[:, 2:3], ALU.subtract, r=[r_ls], w=[r_lam])
    B.ts(neglam[:, 0:1], neglam[:, 0:1], -lam_init, None, ALU.add, None, r=[r_lam], w=[r_lam])
    B.end_scope()

    if STOP == 'setup':
        return finish()

    def load_mod(names):
        out = {}
        gB = {}
        for key, slot, row, kind in names:
            t, r = B.TR("mod_" + key, [128, D], F32)
            B.ld(t[:], modscr[row:row + 1, slot * D:(slot + 1) * D].partition_broadcast(128), w=[r], r=[r_modscr])
            if kind in ("A1", "A2"):
                if kind not in gB:
                    gt, gr = B.r_junk.next()
                    B.ld(gt[:], (g1 if kind == "A1" else g2).partition_broadcast(128), w=[gr])
                    gB[kind] = (gt, gr)
                gt, gr = gB[kind]
                B.stt(t[:], t[:], 1.0, gt[:], ALU.add, ALU.mult, r=[r, gr], w=[r])
            elif kind == "half":
                B.ts(t[:], t[:], 0.5, None, ALU.mult, None, r=[r], w=[r])
            out[key] = (t, r)
        return out

    B.scope()
    M = load_mod([("A1", 1, 0, "A1"), ("sh1", 0, 0, "raw"), ("A1c", 1, 1, "A1"), ("sh1c", 0, 1, "raw")])
    Wkb, rWkb = B.load_w_bf16("Wkb", w_in[:, 1024:1536], 8, 512)
    Wkbs, rWkbs = B.load_w_bf16("Wkbs", w_sw[:, 0:512], 8, 512)
    Wvb, rWvb = B.load_w_bf16("Wvb", w_in[:, 1536:2048], 8, 512)
    x_ring = B.ring("x1a", 3, [128, D], F32)
    hb_ring = B.ring("hb1a", 2, [128, D], BF16)
    hTg_ring = B.ring("hTg", 2, [128, 8, 512], BF16)
    rope_ring = B.ring("rope1a", 2, [128, 2, 512], F32)
    t12_ring = B.ring("t12", 2, [128, 2, 512], F32)
    kt_ring = B.ring("ktout", 3, [128, 512], BF16)
    vst_ring = B.ring("vst", 2, [128, 4, 4, 129], BF16)
    for (t, r) in vst_ring.items:
        B.memset(t[:], 1.0, w=[r], eng="pool")
    for g in range(33):
        ntile = 2 if g == 0 else 4
        kt0 = 0 if g == 0 else 2 + (g - 1) * 4
        ncol = ntile * 128
        tok0 = kt0 * 128
        hTg, rhTg = hTg_ring.next()
        for j in range(ntile):
            xt, rx = x_ring.next()
            if g == 0:
                B.ld(xt[:], ctx[j * 128:(j + 1) * 128, :], w=[rx])
                A, sh = M["A1c"], M["sh1c"]
            else:
                row = ((g - 1) * 4 + j) * 128
                B.ld(xt[:], xfull[row:row + 128, :], w=[rx])
                A, sh = M["A1"], M["sh1"]
            hb, rhb = hb_ring.next()
            B.norm_mod(xt[:], rx, A[0], A[1], sh[0], sh[1], hb[:], rhb)
            bk, rb = br_tp.next()
            B.transpose_to(hb, rhb, 8, hTg[:, :, j * 128:(j + 1) * 128], rhTg, bk, rb, evac="act")
        rp, rrp = rope_ring.next()
        B.ld(rp[:, 0, 0:ncol], ropeF[0, :, tok0:tok0 + ncol], w=[rrp])
        B.ld(rp[:, 1, 0:ncol], ropeF[1, :, tok0:tok0 + ncol], w=[rrp])
        for h in range(4):
            p1, rp1 = br_a.next()
            for k in range(8):
                B.mm(p1[:, 0:ncol], Wkb[:, k, h * 128:(h + 1) * 128], hTg[:, k, 0:ncol], k == 0, k == 7, r=[rWkb, rhTg], w=[rp1])
            p2, rp2 = br_b.next()
            for k in range(8):
                B.mm(p2[:, 0:ncol], Wkbs[:, k, h * 128:(h + 1) * 128], hTg[:, k, 0:ncol], k == 0, k == 7, r=[rWkbs, rhTg], w=[rp2])
            t12, rt12 = t12_ring.next()
            B.tt(t12[:, 0, 0:ncol], p1[:, 0:ncol], rp[:, 0, 0:ncol], ALU.mult, r=[rp1, rrp], w=[rt12])
            B.tt(t12[:, 1, 0:ncol], p2[:, 0:ncol], rp[:, 1, 0:ncol], ALU.mult, r=[rp2, rrp], w=[rt12])
            kto, rkto = kt_ring.next()
            B.tt(kto[:, 0:ncol], t12[:, 0, 0:ncol], t12[:, 1, 0:ncol], ALU.add, r=[rt12], w=[rkto])
            B.st(KbT[h, :, tok0:tok0 + ncol], kto[:, 0:ncol], r=[rkto], w=[r_KbT])
        vst, rvst = vst_ring.next()
        for j in range(ntile):
            pv, rpv = br_a.next()
            for k in range(8):
                B.mm(pv[:, :], hTg[:, k, j * 128:(j + 1) * 128], Wvb[:, k, :], k == 0, k == 7, r=[rWvb, rhTg], w=[rpv])
            B.copy(vst[:, j, :, 0:128], pv[:, :].rearrange("p (h d) -> p h d", h=4), r=[rpv], w=[rvst], eng="act")
        for h in range(4):
            B.st(Vb[h, :, kt0:kt0 + ntile, :], vst[:, 0:ntile, h, :], r=[rvst], w=[r_Vb])
    B.end_scope()

    if STOP == '1a':
        return finish()
    B.scope()
    kaT, r_kaT = B.TR("kaT", [128, 4, NHC * 128], BF16)
    va, r_va = B.TR("va", [128, NHC, 8, 65], BF16)
    B.memset(va[:], 1.0, w=[r_va], eng="pool")
    B.scope()
    M = load_mod([("A1", 1, 0, "A1"), ("sh1", 0, 0, "raw"), ("A1c", 1, 1, "A1"), ("sh1c", 0, 1, "raw")])
    Wka, rWka = B.load_w_bf16("Wka", w_in[:, 0:512], 8, 512)
    Wva, rWva = B.load_w_bf16("Wva", w_in[:, 512:1024], 8, 512)
    x_ring = B.ring("x1b", 3, [128, D], F32)
    hb_ring = B.ring("hb1b", 2, [128, D], BF16)
    for t in range(NHC):
        xt, rx = x_ring.next()
        if t < NHALO:
            B.ld(xt[:], xhalo[t * 128:(t + 1) * 128, :], w=[rx])
            A, sh = M["A1"], M["sh1"]
        else:
            B.ld(xt[:], ctx[(t - NHALO) * 128:(t - NHALO + 1) * 128, :], w=[rx])
            A, sh = M["A1c"], M["sh1c"]
        hb, rhb = hb_ring.next()
        B.norm_mod(xt[:], rx, A[0], A[1], sh[0], sh[1], hb[:], rhb)
        bk, rb = br_tp.next()
        B.transpose_to(hb, rhb, 8, hT_hc[:, :, t * 128:(t + 1) * 128], r_hT, bk, rb, evac="act")
        pv, rpv = br_a.next()
        for k in range(8):
            B.mm(pv[:, :], hT_hc[:, k, t * 128:(t + 1) * 128], Wva[:, k, :], k == 0, k == 7, r=[rWva, r_hT], w=[rpv])
        B.copy(va[:, t, :, 0:64], pv[:, :].rearrange("p (h d) -> p h d", h=8), r=[rpv], w=[r_va], eng="act")
    for c0 in range(0, NHC * 128, 512):
        ncol = min(512, NHC * 128 - c0)
        for hp in range(4):
            p1, rp1 = br_b.next()
            for k in range(8):
                B.mm(p1[:, 0:ncol], Wka[:, k, hp * 128:(hp + 1) * 128], hT_hc[:, k, c0:c0 + ncol], k == 0, k == 7, r=[rWka, r_hT], w=[rp1])
            B.copy(kaT[:, hp, c0:c0 + ncol], p1[:, 0:ncol], r=[rp1], w=[r_kaT])
    B.end_scope()

    if STOP == '1b':
        return finish()

    def qcol(qi):
        return (qi + 2) * 128 if qi < NLT else (NHALO + qi - NLT) * 128

    def store_yT(b, ytile, rytile, qi, nchunk=4, chunk0=0):
        bk, rb = br_tp.next()
        yst, ryst = yst_ring.next()
        B.transpose_to(ytile, rytile, nchunk, yst[:, 0:nchunk, :], ryst, bk, rb)
        B.st(yT[b, :, chunk0:chunk0 + nchunk, qi * 128:(qi + 1) * 128], yst[:, 0:nchunk, :], r=[ryst], w=[r_yT[b]])

    B.scope()
    yst_ring = B.ring("yst", 2, [128, 4, 128], BF16)
    Wqa, rWqa = B.load_w_bf16("Wqa", w_in[:, 2048:2560], 8, 512)
    qm_ring = B.ring("qm", 2, [128, 8, 128], BF16)
    for (t, r) in qm_ring.items:
        B.memset(t[:], 0.0, w=[r], eng="pool")
    nb_ring = B.ring("nb", 3, [128, 768], F32)
    sl_ring = B.ring("sl", 2, [128, 768], F32)
    e_ring = B.ring("ena", 2, [128, 1024], BF16)
    ya_ring = B.ring("ya", 2, [128, 512], BF16)
    rd_ring = B.ring("rdna", 3, [128, 2], F32)
    br_s = B.bank_ring([0, 1, 2, 3])
    br_acc = B.bank_ring([4, 5])
    for qi in range(NQ):
        c0 = qcol(qi)
        bk, rb = br_s.next()
        for hp in range(4):
            for k in range(8):
                B.mm(bk[:, hp * 128:(hp + 1) * 128], Wqa[:, k, hp * 128:(hp + 1) * 128], hT_hc[:, k, c0:c0 + 128], k == 0, k == 7,
                     r=[rWqa, r_hT], w=[rb])
        qm, rqm = qm_ring.next()
        bk3 = bk[:, :].rearrange("p (a t) -> p a t", a=4)
        B.ts(qm[0:64, 0:8:2, :], bk3[0:64, :, :], 0.125, None, ALU.mult, None, r=[rb], w=[rqm])
        B.ts(qm[64:128, 1:8:2, :], bk3[64:128, :, :], 0.125, None, ALU.mult, None, r=[rb], w=[rqm])
        ya, rya = ya_ring.next()
        if qi < NLT:
            if qi < 2:
                loc = list(range(qi, qi + 6))
            elif qi >= NLT - 2:
                loc = list(range(qi - 1, qi + 5))
            else:
                loc = list(range(qi, qi + 5))
            keys = [NHALO, NHALO + 1] + loc
        else:
            loc = []
            keys = [NHALO, NHALO + 1]
        nl = len(loc)
        for h in range(8):
            hp = h // 2
            sA, rsA = br_s.next()
            sB, rsB = (br_s.next() if nl > 2 else (None, None))
            for j, kt in enumerate(keys):
                bank, rbank = (sA, rsA) if j < 4 else (sB, rsB)
                jj = j % 4
                B.mm(bank[:, jj * 128:(jj + 1) * 128], kaT[:, hp, kt * 128:(kt + 1) * 128], qm[:, h, :], True, True, r=[r_kaT, rqm], w=[rbank])
            E, rE = e_ring.next()
            B.act(E[:, 0:256], sA[:, 0:256], AF.Exp, r=[rsA], w=[rE])
            if nl:
                nb, rnb = nb_ring.next()
                B.ld(nb[:, 0:nl * 128], nabias[qi, h, :, 0:nl * 128], w=[rnb])
                sl, rsl = sl_ring.next()
                B.tt(sl[:, 0:256], sA[:, 256:512], nb[:, 0:256], ALU.add, r=[rsA, rnb], w=[rsl])
                B.tt(sl[:, 256:nl * 128], sB[:, 0:(nl - 2) * 128], nb[:, 256:nl * 128], ALU.add, r=[rsB, rnb], w=[rsl])
                B.act(E[:, 256:256 + nl * 128], sl[:, 0:nl * 128], AF.Exp, r=[rsl], w=[rE])
            pa, rpa = br_acc.next()
            for j, kt in enumerate(keys):
                B.mm(pa[:, 0:65], E[:, j * 128:(j + 1) * 128], va[:, kt, h, :], j == 0, j == len(keys) - 1, r=[rE, r_va], w=[rpa])
            rd, rrd = rd_ring.next()
            kb.op("dve", lambda e, rd=rd, pa=pa: e.reciprocal(out=rd[:, 0:1], in_=pa[:, 64:65]), r=[rpa], w=[rrd])
            B.ts(ya[:, h * 64:(h + 1) * 64], pa[:, 0:64], rd[:, 0:1], None, ALU.mult, None, r=[rpa, rrd], w=[rya])
        store_yT(0, ya, rya, qi)
    B.end_scope()
    B.end_scope()

    if STOP == 'B':
        return finish()
    B.scope()
    yst_ring = B.ring("ystc", 2, [128, 4, 128], BF16)
    Wu, rWu = B.load_w_bf16("Wu", w_in[:, 3072:3584], 8, 512)
    Wv, rWv = B.load_w_bf16("Wv", w_in[:, 3584:4096], 8, 512)
    wsb, r_wsb = B.TR("wsb", [128, 4, 128], BF16)
    B.ld(wsb[:], wsT.rearrange("g q p -> q g p"), w=[r_wsb], q="pool")
    bst, r_bst = B.TR("bst", [128, 4], F32)
    B.ld(bst[:], bsT, w=[r_bst])
    lng, r_lng = B.bcast("lng", ln_g, 512)
    lnb, r_lnb = B.bcast("lnb", ln_b, 512)
    gt_ring = B.ring("gtmp", 4, [128, 512], F32)
    gu_ring = B.ring("gu", 2, [128, 512], F32)
    gv_ring = B.ring("gv", 2, [128, 512], F32)
    vn_ring = B.ring("vnb", 2, [128, 512], BF16)
    yc_ring = B.ring("yc", 2, [128, 512], BF16)
    bn_ring = B.ring("bn", 2, [128, 12], F32)
    br_m = B.bank_ring([4, 5])

    def gelu_from_psum(ps, rps, out, rout):
        x2, rx2 = gt_ring.next()
        B.act(x2[:], ps[:, :], AF.Square, r=[rps], w=[rx2], scale=math.sqrt(0.044715))
        z, rz = gt_ring.next()
        B.stt(z[:], x2[:], 1.0, ps[:, :], ALU.add, ALU.mult, r=[rx2, rps], w=[rz])
        B.act(z[:], z[:], AF.Tanh, r=[rz], w=[rz], scale=0.7978845608028654)
        B.act(x2[:], ps[:, :], AF.Copy, r=[rps, rx2], w=[rx2], scale=0.5)
        B.stt(out[:], z[:], 1.0, x2[:], ALU.add, ALU.mult, r=[rz, rx2], w=[rout])

    for qi in range(NQ):
        c0 = qcol(qi)
        pu, rpu = br_a.next()
        for k in range(8):
            B.mm(pu[:, :], hT_hc[:, k, c0:c0 + 128], Wu[:, k, :], k == 0, k == 7, r=[rWu, r_hT], w=[rpu])
        pvv, rpvv = br_a.next()
        for k in range(8):
            B.mm(pvv[:, :], hT_hc[:, k, c0:c0 + 128], Wv[:, k, :], k == 0, k == 7, r=[rWv, r_hT], w=[rpvv])
        gu, rgu = gu_ring.next()
        gv, rgv = gv_ring.next()
        gelu_from_psum(pu, rpu, gu, rgu)
        gelu_from_psum(pvv, rpvv, gv, rgv)
        bn, rbn = bn_ring.next()
        kb.op("dve", lambda e, bn=bn, gv=gv: e.bn_stats(out=bn[:, 0:6], in_=gv[:]), r=[rgv], w=[rbn])
        kb.op("dve", lambda e, bn=bn: e.bn_aggr(out=bn[:, 6:8], in_=bn[:, 0:6]), r=[rbn], w=[rbn])
        B.ts(bn[:, 8:9], bn[:, 7:8], EPS, None, ALU.add, None, r=[rbn], w=[rbn])
        kb.op("pool", lambda e, bn=bn: e.tensor_tensor(out=bn[:, 9:10], in0=bn[:, 8:9], in1=B.mhalf[:, 0:1], op=ALU.pow), r=[rbn, B.r_id], w=[rbn])
        B.ts(gv[:], gv[:], bn[:, 6:7], bn[:, 9:10], ALU.subtract, ALU.mult, r=[rgv, rbn], w=[rgv])
        B.tt(gv[:], gv[:], lng[:], ALU.mult, r=[rgv, r_lng], w=[rgv])
        vn, rvn = vn_ring.next()
        B.tt(vn[:], gv[:], lnb[:], ALU.add, r=[rgv, r_lnb], w=[rvn])
        pm, rpm = br_m.next()
        for g in range(4):
            B.mm(pm[:, g * 128:(g + 1) * 128], wsb[:, g, :], vn[:, g * 128:(g + 1) * 128], True, True, r=[r_wsb, rvn], w=[rpm])
        yc, ryc = yc_ring.next()
        for g in range(4):
            B.stt(yc[:, g * 128:(g + 1) * 128], pm[:, g * 128:(g + 1) * 128], bst[:, g:g + 1], gu[:, g * 128:(g + 1) * 128], ALU.add, ALU.mult,
                  r=[rpm, r_bst, rgu], w=[ryc])
        store_yT(2, yc, ryc, qi)
    B.end_scope()

    if STOP == 'C':
        return finish()
    B.scope()
    yst_ring = B.ring("ysta", 2, [128, 4, 128], BF16)
    KTh, r_KTh = B.TR("KTh", [128, L + CTX], BF16)
    Vh, r_Vh = B.TR("Vh", [128, NKT, 129], BF16)
    QT = [B.TR("QT%d" % m, [128, NQT], BF16) for m in range(2)]
    for (t, r) in QT:
        B.memset(t[:], 0.0, w=[r], eng="pool")
    wq_ring = B.ring("wqb", 2, [128, 8, 256], BF16)
    ropeq, r_ropeq = B.TR("ropeq", [128, 2, NQT], F32)
    B.ld(ropeq[:, 0, :], ropeQ[0, :, 0:NQT], w=[r_ropeq])
    B.ld(ropeq[:, 1, :], ropeQ[1, :, 0:NQT], w=[r_ropeq])
    sgB, r_sgB = B.bcast("sgB", subg, 128)
    B.ts(sgB[:], sgB[:], 1.0 - lam_init, None, ALU.mult, None, r=[r_sgB], w=[r_sgB])
    t12_ring = B.ring("t12a", 2, [128, 2, 512], F32)
    e_ring = B.ring("eda", 3, [128, 512], BF16)
    om_ring = B.ring("om", 4, [128, 129], F32)
    o_ring = B.ring("oda", 2, [128, 128], F32)
    st_ring = B.ring("stda", 3, [128, 8], F32)
    yb_ring = B.ring("yb", 2, [128, 128], BF16)
    br_s = B.bank_ring([0, 1, 2])
    br_acc = B.bank_ring([3, 4])
    br_q = B.bank_ring([5, 6])
    br_tp1 = B.bank_ring([7])
    for h in range(4):
        for c0 in range(0, L + CTX, 4160):
            B.ld(KTh[:, c0:c0 + 4160], KbT[h, :, c0:c0 + 4160], r=[r_KbT], w=[r_KTh])
        for t0 in range(0, NKT, 26):
            B.ld(Vh[:, t0:t0 + 26, :], Vb[h, :, t0:t0 + 26, :], r=[r_Vb], w=[r_Vh])
        wq, rwq = wq_ring.next()
        B.ld(wq[:, :, 0:128], w_in[:, 2560 + h * 128:2560 + (h + 1) * 128].rearrange("(k p) n -> p k n", p=128), w=[rwq], q="pool")
        B.ld(wq[:, :, 128:256], w_sw[:, 512 + h * 128:512 + (h + 1) * 128].rearrange("(k p) n -> p k n", p=128), w=[rwq], q="pool")
        for q0 in range(0, NQT, 512):
            ncol = min(512, NQT - q0)
            hc0 = qcol(q0 // 128)
            p1, rp1 = br_q.next()
            for k in range(8):
                B.mm(p1[:, 0:ncol], wq[:, k, 0:128], hT_hc[:, k, hc0:hc0 + ncol], k == 0, k == 7, r=[rwq, r_hT], w=[rp1])
            p2, rp2 = br_q.next()
            for k in range(8):
                B.mm(p2[:, 0:ncol], wq[:, k, 128:256], hT_hc[:, k, hc0:hc0 + ncol], k == 0, k == 7, r=[rwq, r_hT], w=[rp2])
            t12, rt12 = t12_ring.next()
            B.tt(t12[:, 0, 0:ncol], p1[:, 0:ncol], ropeq[:, 0, q0:q0 + ncol], ALU.mult, r=[rp1, r_ropeq], w=[rt12])
            B.tt(t12[:, 1, 0:ncol], p2[:, 0:ncol], ropeq[:, 1, q0:q0 + ncol], ALU.mult, r=[rp2, r_ropeq], w=[rt12])
            B.tt(t12[:, 0, 0:ncol], t12[:, 0, 0:ncol], t12[:, 1, 0:ncol], ALU.add, r=[rt12], w=[rt12])
            B.ts(QT[0][0][0:64, q0:q0 + ncol], t12[0:64, 0, 0:ncol], 0.125, None, ALU.mult, None, r=[rt12], w=[QT[0][1]])
            B.ts(QT[1][0][64:128, q0:q0 + ncol], t12[64:128, 0, 0:ncol], 0.125, None, ALU.mult, None, r=[rt12], w=[QT[1][1]])
        for qi in range(NQ):
            nkt = NKT if qi < NLT else 2
            oms = []
            for m in range(2):
                qt, rqt = QT[m]
                pa, rpa = br_acc.next()
                for k0 in range(0, nkt, 4):
                    nk = min(4, nkt - k0)
                    sb_, rsb = br_s.next()
                    for j in range(nk):
                        kt = k0 + j
                        B.mm(sb_[:, j * 128:(j + 1) * 128], KTh[:, kt * 128:(kt + 1) * 128], qt[:, qi * 128:(qi + 1) * 128], True, True,
                             r=[r_KTh, rqt], w=[rsb])
                    E, rE = e_ring.next()
                    B.act(E[:, 0:nk * 128], sb_[:, 0:nk * 128], AF.Exp, r=[rsb], w=[rE])
                    for j in range(nk):
                        kt = k0 + j
                        B.mm(pa[:, 0:129], E[:, j * 128:(j + 1) * 128], Vh[:, kt, :], kt == 0, kt == nkt - 1, r=[rE, r_Vh], w=[rpa])
                om, rom = om_ring.next()
                B.copy(om[:], pa[:, 0:129], r=[rpa], w=[rom], eng="act")
                oms.append((om, rom))
            (o0, ro0), (o1, ro1) = oms
            stt_, rst = st_ring.next()
            kb.op("dve", lambda e, s=stt_, o=o0: e.reciprocal(out=s[:, 0:1], in_=o[:, 128:129]), r=[ro0], w=[rst])
            kb.op("dve", lambda e, s=stt_, o=o1: e.reciprocal(out=s[:, 1:2], in_=o[:, 128:129]), r=[ro1], w=[rst])
            B.tt(stt_[:, 1:2], stt_[:, 1:2], neglam[:, 0:1], ALU.mult, r=[rst, r_lam], w=[rst])
            o, ro = o_ring.next()
            B.ts(o[:], o0[:, 0:128], stt_[:, 0:1], None, ALU.mult, None, r=[ro0, rst], w=[ro])
            B.stt(o[:], o1[:, 0:128], stt_[:, 1:2], o[:], ALU.mult, ALU.add, r=[ro1, rst, ro], w=[ro])
            B.act(o0[:, 0:128], o[:], AF.Square, r=[ro, ro0], w=[ro0, rst], accum_out=stt_[:, 2:3])
            B.ts(stt_[:, 3:4], stt_[:, 2:3], 1.0 / 128, EPS, ALU.mult, ALU.add, r=[rst], w=[rst])
            kb.op("pool", lambda e, s=stt_: e.tensor_tensor(out=s[:, 4:5], in0=s[:, 3:4], in1=B.mhalf[:, 0:1], op=ALU.pow), r=[rst, B.r_id], w=[rst])
            yb, ryb = yb_ring.next()
            B.stt(yb[:], o[:], stt_[:, 4:5], sgB[:], ALU.mult, ALU.mult, r=[ro, rst, r_sgB], w=[ryb])
            bk, rb = br_tp1.next()
            yst, ryst = yst_ring.next()
            B.transpose_to(yb, ryb, 1, yst[:, 0:1, :], ryst, bk, rb)
            B.st(yT[1, :, h:h + 1, qi * 128:(qi + 1) * 128], yst[:, 0:1, :], r=[ryst], w=[r_yT[1]])
    B.end_scope()

    if STOP == 'A':
        return finish()
    B.scope()
    M = load_mod([("gt1", 2, 0, "half"), ("A2", 4, 0, "A2"), ("sh2", 3, 0, "raw")] +
                 ([] if last else [("gt1c", 2, 1, "half"), ("A2c", 4, 1, "A2"), ("sh2c", 3, 1, "raw")]))
    Wg, rWg = B.load_w_bf16("Wg", w_in[:, 4096:7168], 8, 3072)
    Wbr, rWbr = B.load_w_bf16("Wbr", w_br, 12, 1024)
    Wo, rWo = B.load_w_bf16("Wo", w_out, 8, 1024)
    Wr, rWr = B.TR("Wr", [128, 8, 16], F32)
    B.ld(Wr[:], w_r.rearrange("(k p) n -> p k n", p=128), w=[rWr])
    yt_ring = B.ring("ytd", 2, [128, 3, 4, 128], BF16)
    x_ring = B.ring("xd", 2, [128, D], F32)
    thg, r_thg = B.TR("thg", [128, 3072], BF16)
    macc, r_macc = B.TR("macc", [128, D], F32)
    mtmp, r_mtmp = B.TR("mtmp", [128, D], F32)
    mb, r_mb = B.TR("mb", [128, D], BF16)
    mT, r_mT = B.TR("mT", [128, 8, 128], BF16)
    h2T, r_h2T = mtmp[:].rearrange("p (k t) -> p k t", k=8), r_mtmp
    sm_ring = B.ring("smx", 2, [128, 40], F32)
    for qi in range(NQ):
        c0 = qcol(qi)
        lat = qi < NLT
        yt, ryt = yt_ring.next()
        for b in range(3):
            B.ld(yt[:, b, :, :], yT[b, :, :, qi * 128:(qi + 1) * 128], r=[r_yT[b]], w=[ryt])
        xt, rx = x_ring.next()
        if lat:
            B.ld(xt[:], xhalo[c0:c0 + 128, :], w=[rx])
        else:
            B.ld(xt[:], ctx[(qi - NLT) * 128:(qi - NLT + 1) * 128, :], w=[rx])
        for nb in range(6):
            pg, rpg = br_a.next()
            for k in range(8):
                B.mm(pg[:, :], hT_hc[:, k, c0:c0 + 128], Wg[:, k, nb * 512:(nb + 1) * 512], k == 0, k == 7, r=[rWg, r_hT], w=[rpg])
            B.act(thg[:, nb * 512:(nb + 1) * 512], pg[:, :], AF.Tanh, r=[rpg], w=[r_thg], scale=0.5)
        for b in range(3):
            for half in range(2):
                pb, rpb = br_b.next()
                for k in range(4):
                    B.mm(pb[:, :], yt[:, b, k, :], Wbr[:, b * 4 + k, half * 512:(half + 1) * 512], k == 0, k == 3, r=[ryt, rWbr], w=[rpb])
                gsl = thg[:, b * 1024 + half * 512:b * 1024 + (half + 1) * 512]
                msl = macc[:, half * 512:(half + 1) * 512]
                if b == 0:
                    B.stt(msl, gsl, 1.0, pb[:, :], ALU.add, ALU.mult, r=[r_thg, rpb], w=[r_macc])
                else:
                    tsl = mtmp[:, half * 512:(half + 1) * 512]
                    B.stt(tsl, gsl, 1.0, pb[:, :], ALU.add, ALU.mult, r=[r_thg, rpb], w=[r_mtmp])
                    if b == 1:
                        B.tt(msl, msl, tsl, ALU.add, r=[r_macc, r_mtmp], w=[r_macc])
                    else:
                        B.tt(mb[:, half * 512:(half + 1) * 512], msl, tsl, ALU.add, r=[r_macc, r_mtmp], w=[r_mb])
        bk, rb = br_tp.next()
        B.transpose_to(mb, r_mb, 8, mT[:], r_mT, bk, rb)
        gt = M["gt1"] if lat else M["gt1c"]
        for half in range(2):
            po, rpo = br_b.next()
            for k in range(8):
                B.mm(po[:, :], mT[:, k, :], Wo[:, k, half * 512:(half + 1) * 512], k == 0, k == 7, r=[r_mT, rWo], w=[rpo])
            sl_ = slice(half * 512, (half + 1) * 512)
            B.tt(mtmp[:, sl_], po[:, :], gt[0][:, sl_], ALU.mult, r=[rpo, gt[1], r_mtmp], w=[r_mtmp])
            B.tt(xt[:, sl_], xt[:, sl_], mtmp[:, sl_], ALU.add, r=[rx, r_mtmp], w=[rx])
        fin.append(B.st(xmid[qi * 128:(qi + 1) * 128, :], xt[:], r=[rx]))
        A2, sh2 = (M["A2"], M["sh2"]) if lat else (M["A2c"], M["sh2c"])
        B.norm_mod(xt[:], rx, A2[0], A2[1], sh2[0], sh2[1], macc[:], r_macc)
        for hf in range(2):
            bk, rb = br_a.next()
            for k in range(4):
                B.tr(bk[:, k * 128:(k + 1) * 128], macc[:, (hf * 4 + k) * 128:(hf * 4 + k + 1) * 128], B.identf[:], r=[r_macc, B.r_id], w=[rb])
            B.copy(h2T[:, hf * 4:(hf + 1) * 4, :], bk[:, :].rearrange("p (k t) -> p k t", k=4), r=[rb, r_h2T], w=[r_h2T], eng=("act" if hf else "dve"))
        pl, rpl = br_b.next()
        for k in range(8):
            B.mm(pl[:, 0:16], h2T[:, k, :], Wr[:, k, :], k == 0, k == 7, r=[r_h2T, rWr], w=[rpl])
        sm, rsm = sm_ring.next()
        kb.op("dve", lambda e, sm=sm, pl=pl: e.tensor_reduce(out=sm[:, 0:1], in_=pl[:, 0:16], axis=AX.X, op=ALU.max), r=[rpl], w=[rsm])
        B.ts(sm[:, 1:2], sm[:, 0:1], -1.0, None, ALU.mult, None, r=[rsm], w=[rsm])
        B.act(sm[:, 8:24], pl[:, 0:16], AF.Exp, r=[rpl, rsm], w=[rsm], bias=sm[:, 1:2], accum_out=sm[:, 2:3])
        kb.op("dve", lambda e, sm=sm: e.reciprocal(out=sm[:, 3:4], in_=sm[:, 2:3]), r=[rsm], w=[rsm])
        B.ts(sm[:, 24:40], sm[:, 8:24], sm[:, 3:4], None, ALU.mult, None, r=[rsm], w=[rsm])
        fin.append(B.st(aff[qi * 128:(qi + 1) * 128, :], sm[:, 24:40], r=[rsm]))
    kb.barrier()
    kb.flush(final_wait_ops=fin)
    B.stack.pop().close()
    B.stack.pop().close()
    return nc


NE = 16


def build_moe(last):
    B = Builder("moe")
    nc, kb = B.nc, B.kb
    NT = NLT + (0 if last else 1)
    NTT = NT * 128
    xin = B.din("xin", [NTT, D])
    affall = B.din("affall", [128, 128 * NE])
    affc = B.din("affc", [128, 2 * NE])
    affown = B.din("affown", [NTT, NE])
    modin = B.din("modin", [2, 6 * D])
    g2 = B.din("g2", [1, D])
    gfin = B.din("gfin", [1, D])
    w1 = B.din("w1", [NE, D, D])
    w3 = B.din("w3", [NE, D, D])
    w2 = B.din("w2", [NE, D, D])
    xout = B.dout("xout", [NTT, D])
    fin = []

    B.scope()
    B.consts()
    h2T, r_h2T = B.TR("h2T", [128, 8, NTT], BF16)
    acc = B.T("acc", [128, NT, D], F32)
    r_acc = [kb.res() for _ in range(NT)]
    Gm, r_Gm = B.TR("Gm", [128, NT, NE], F32)
    thr, r_thr = B.TR("thr", [128, 2 * NE], F32)
    Wt = {}
    for nm in ("w1", "w3", "w2"):
        Wt[nm] = B.TR("W" + nm, [128, 8, D], BF16)
    srcs = {"w1": w1, "w3": w3, "w2": w2}

    def load_expert(nm, e):
        t, r = Wt[nm]
        src = srcs[nm][e].rearrange("(k p) n -> p k n", p=128)
        for k0 in range(0, 8, 4):
            B.ld(t[:, k0:k0 + 4, :], src[:, k0:k0 + 4, :], w=[r], q="pool")

    br_a = B.bank_ring([0, 1, 2])
    br_b = B.bank_ring([3, 4, 5])
    br_tp = B.bank_ring([6, 7])

    B.scope()
    A_, rA = B.TR("affL", [128, 128 * NE], F32)
    C_, rC = B.TR("affC", [128, 2 * NE], F32)
    B.ld(A_[:], affall, w=[rA])
    B.ld(C_[:], affc, w=[rC])
    ge, rge = B.TR("ge", [128, 128 * NE], F32)
    gec, rgec = B.TR("gec", [128, 2 * NE], F32)
    lo, rlo = B.TR("lo", [128, 2 * NE], F32)
    hi, rhi = B.TR("hi", [128, 2 * NE], F32)
    mid, rmid = B.TR("mid", [128, 2 * NE], F32)
    cnt, rcnt = B.TR("cnt", [128, 2 * NE], F32)
    tgt, rtgt = B.TR("tgt", [128, 2 * NE], F32)
    pm_, rpm_ = B.TR("pmask", [128, 2 * NE], U32)
    nm_, rnm_ = B.TR("nmask", [128, 2 * NE], U32)
    ones, rones = B.TR("ones", [128, 128], F32)
    B.memset(ones[:], 1.0, w=[rones])
    B.memset(lo[:], 0.0, w=[rlo])
    B.memset(hi[:], 2.0, w=[rhi])
    B.memset(tgt[:, 0:NE], float(2 * L // NE), w=[rtgt])
    B.memset(tgt[:, NE:2 * NE], float(2 * CTX // NE), w=[rtgt])
    A3 = A_[:].rearrange("p (j e) -> p j e", e=NE)
    C3 = C_[:].rearrange("p (j e) -> p j e", e=NE)
    for it in range(34):
        B.tt(mid[:], lo[:], hi[:], ALU.add, r=[rlo, rhi], w=[rmid])
        B.ts(mid[:], mid[:], 0.5, None, ALU.mult, None, r=[rmid], w=[rmid])
        B.tt(ge[:].rearrange("p (j e) -> p j e", e=NE), A3, mid[:, 0:NE].unsqueeze(1).broadcast_to([128, 128, NE]), ALU.is_ge, r=[rA, rmid], w=[rge])
        kb.op("dve", lambda e: e.tensor_reduce(out=cnt[:, 0:NE], in_=ge[:].rearrange("p (j e) -> p e j", e=NE), axis=AX.X, op=ALU.add), r=[rge], w=[rcnt])
        B.tt(gec[:].rearrange("p (j e) -> p j e", e=NE), C3, mid[:, NE:2 * NE].unsqueeze(1).broadcast_to([128, 2, NE]), ALU.is_ge, r=[rC, rmid], w=[rgec])
        kb.op("dve", lambda e: e.tensor_reduce(out=cnt[:, NE:2 * NE], in_=gec[:].rearrange("p (j e) -> p e j", e=NE), axis=AX.X, op=ALU.add), r=[rgec, rcnt], w=[rcnt])
        bk, rb = br_a.next()
        B.mm(bk[:, 0:2 * NE], ones[:], cnt[:], True, True, r=[rones, rcnt], w=[rb])
        B.tt(pm_[:], bk[:, 0:2 * NE], tgt[:], ALU.is_ge, r=[rb, rtgt], w=[rpm_])
        B.tt(nm_[:], bk[:, 0:2 * NE], tgt[:], ALU.is_lt, r=[rb, rtgt], w=[rnm_])
        kb.op("dve", lambda e: e.copy_predicated(out=lo[:], mask=pm_[:], data=mid[:]), r=[rpm_, rmid, rlo], w=[rlo])
        kb.op("dve", lambda e: e.copy_predicated(out=hi[:], mask=nm_[:], data=mid[:]), r=[rnm_, rmid, rhi], w=[rhi])
    B.copy(thr[:], lo[:], r=[rlo], w=[r_thr])
    ao, rao = B.TR("ao", [128, NT, NE], F32)
    B.ld(ao[:], affown.rearrange("(t p) e -> p t e", p=128), w=[rao])
    B.tt(Gm[:, 0:NLT, :], ao[:, 0:NLT, :], thr[:, 0:NE].unsqueeze(1).broadcast_to([128, NLT, NE]), ALU.is_ge, r=[rao, r_thr], w=[r_Gm])
    if not last:
        B.tt(Gm[:, NLT:NT, :], ao[:, NLT:NT, :], thr[:, NE:2 * NE].unsqueeze(1).broadcast_to([128, 1, NE]), ALU.is_ge, r=[rao, r_thr], w=[r_Gm])
    B.tt(Gm[:], Gm[:], ao[:], ALU.mult, r=[r_Gm, rao], w=[r_Gm])
    B.end_scope()

    B.scope()
    B.norm_setup()
    def modrow(key, slot, row, kind):
        t, r = B.TR("mod_" + key, [128, D], F32)
        B.ld(t[:], modin[row:row + 1, slot * D:(slot + 1) * D].partition_broadcast(128), w=[r])
        return t, r
    g2B = B.bcast("g2B", g2, D)
    A2 = modrow("A2", 4, 0, None); sh2 = modrow("sh2", 3, 0, None)
    B.stt(A2[0][:], A2[0][:], 1.0, g2B[0][:], ALU.add, ALU.mult, r=[A2[1], g2B[1]], w=[A2[1]])
    if not last:
        A2c = modrow("A2c", 4, 1, None); sh2c = modrow("sh2c", 3, 1, None)
        B.stt(A2c[0][:], A2c[0][:], 1.0, g2B[0][:], ALU.add, ALU.mult, r=[A2c[1], g2B[1]], w=[A2c[1]])
    load_expert("w1", 0); load_expert("w3", 0); load_expert("w2", 0)
    hb_ring = B.ring("hbm", 2, [128, D], BF16)
    for t in range(NT):
        xt = acc[:, t, :]
        B.ld(xt, xin[t * 128:(t + 1) * 128, :], w=[r_acc[t]])
        A, sh = (A2, sh2) if t < NLT else (A2c, sh2c)
        hb, rhb = hb_ring.next()
        B.norm_mod(xt, r_acc[t], A[0], A[1], sh[0], sh[1], hb[:], rhb)
        bk, rb = br_tp.next()
        B.transpose_to(hb, rhb, 8, h2T[:, :, t * 128:(t + 1) * 128], r_h2T, bk, rb, evac="act")
    B.end_scope()

    B.scope()
    hidT, r_hid = B.TR("hidT", [128, 8, NTT], BF16)
    sg_ring = B.ring("sgm", 3, [128, 512], F32)
    for e in range(NE):
        (W1, rW1), (W3, rW3), (W2, rW2) = Wt["w1"], Wt["w3"], Wt["w2"]
        for c0 in range(0, NTT, 512):
            ncol = min(512, NTT - c0)
            for f in range(8):
                pa, rpa = br_a.next()
                for k in range(8):
                    B.mm(pa[:, 0:ncol], W1[:, k, f * 128:(f + 1) * 128], h2T[:, k, c0:c0 + ncol], k == 0, k == 7, r=[rW1, r_h2T], w=[rpa])
                pb, rpb = br_b.next()
                for k in range(8):
                    B.mm(pb[:, 0:ncol], W3[:, k, f * 128:(f + 1) * 128], h2T[:, k, c0:c0 + ncol], k == 0, k == 7, r=[rW3, r_h2T], w=[rpb])
                sg, rsg = sg_ring.next()
                B.act(sg[:, 0:ncol], pa[:, 0:ncol], AF.Tanh, r=[rpa], w=[rsg], scale=0.5)
                B.stt(sg[:, 0:ncol], sg[:, 0:ncol], 1.0, pa[:, 0:ncol], ALU.add, ALU.mult, r=[rsg, rpa], w=[rsg])
                B.stt(hidT[:, f, c0:c0 + ncol], sg[:, 0:ncol], 0.5, pb[:, 0:ncol], ALU.mult, ALU.mult, r=[rsg, rpb], w=[r_hid])
        if e + 1 < NE:
            load_expert("w1", e + 1); load_expert("w3", e + 1)
        for t in range(NT):
            for half in range(2):
                py, rpy = (br_a if half == 0 else br_b).next()
                for f in range(8):
                    B.mm(py[:, :], hidT[:, f, t * 128:(t + 1) * 128], W2[:, f, half * 512:(half + 1) * 512], f == 0, f == 7, r=[r_hid, rW2], w=[rpy])
                asl = acc[:, t, half * 512:(half + 1) * 512]
                if e == 0:
                    B.ts(asl, py[:, :], Gm[:, t, e:e + 1], None, ALU.mult, None, r=[rpy, r_Gm, r_acc[t]], w=[r_acc[t]])
                else:
                    B.stt(asl, py[:, :], Gm[:, t, e:e + 1], asl, ALU.mult, ALU.add, r=[rpy, r_Gm, r_acc[t]], w=[r_acc[t]])
        if e + 1 < NE:
            load_expert("w2", e + 1)
    B.end_scope()

    B.scope()
    B.norm_setup()
    def modrow2(key, slot, row):
        t, r = B.TR("modf_" + key, [128, D], F32)
        B.ld(t[:], modin[row:row + 1, slot * D:(slot + 1) * D].partition_broadcast(128), w=[r])
        return t, r
    gt2 = modrow2("gt2", 5, 0)
    if not last:
        gt2c = modrow2("gt2c", 5, 1)
    else:
        gfB = B.bcast("gfB", gfin, D)
    x_ring = B.ring("xf", 2, [128, D], F32)
    for t in range(NT):
        xt, rx = x_ring.next()
        B.ld(xt[:], xin[t * 128:(t + 1) * 128, :], w=[rx])
        gt = gt2 if t < NLT else gt2c
        B.tt(acc[:, t, :], acc[:, t, :], gt[0][:], ALU.mult, r=[r_acc[t], gt[1]], w=[r_acc[t]])
        B.tt(xt[:], xt[:], acc[:, t, :], ALU.add, r=[rx, r_acc[t]], w=[rx])
        if last:
            rstd, rs = B.rstd_of(xt[:], 1024, rx)
            B.stt(xt[:], xt[:], rstd, gfB[0][:], ALU.mult, ALU.mult, r=[rx, rs, gfB[1]], w=[rx])
        fin.append(B.st(xout[t * 128:(t + 1) * 128, :], xt[:], r=[rx]))
    kb.barrier()
    kb.flush(final_wait_ops=fin)
    B.stack.pop().close()
    B.stack.pop().close()
    return nc


def _rope_tables():
    theta = 10000.0
    inv = (theta ** (-np.arange(0, 32, 2, dtype=np.float32) / 32.0)).astype(np.float32)
    t = np.arange(L)
    ang_r = (t // 64).astype(np.float32)[:, None] * inv
    ang_c = (t % 64).astype(np.float32)[:, None] * inv
    cos = np.zeros((L, 64), np.float32)
    ss = np.zeros((L, 64), np.float32)
    for base, ang in ((0, ang_r), (32, ang_c)):
        c, s = np.cos(ang).astype(np.float32), np.sin(ang).astype(np.float32)
        cos[:, base:base + 16] = c
        cos[:, base + 16:base + 32] = c
        ss[:, base:base + 16] = -s
        ss[:, base + 16:base + 32] = s
    cosT = np.ascontiguousarray(np.concatenate([cos.T, cos.T], 0))
    ssT = np.ascontiguousarray(np.concatenate([ss.T, ss.T], 0))
    return cosT, ssT


def _swap_perm():
    p = np.arange(64)
    partner = np.where((p % 32) < 16, p + 16, p - 16)
    return partner


def _na_bias(rpb, c):
    out = np.full((NLT, 8, 128, 6, 128), NEG, np.float32)
    qrow = np.repeat(np.arange(2), 64)
    qcolv = np.tile(np.arange(64), 2)
    for qi in range(NLT):
        T = NLT * c + qi
        if qi < 2:
            loc = list(range(qi, qi + 6))
        elif qi >= NLT - 2:
            loc = list(range(qi - 1, qi + 5))
        else:
            loc = list(range(qi, qi + 5))
        r = 2 * T + qrow
        r0 = np.clip(r - 4, 0, 248)
        cs = np.clip(qcolv - 8, 0, 48)
        for j, u in enumerate(loc):
            Tk = NLT * c - 2 + u
            if Tk < 0 or Tk >= L // 128:
                continue
            kr = 2 * Tk + qrow
            kc = qcolv
            inwin = ((kr[:, None] >= r0[None, :]) & (kr[:, None] < r0[None, :] + 8) &
                     (kc[:, None] >= cs[None, :]) & (kc[:, None] < cs[None, :] + 16))
            dr = np.clip(kr[:, None] - r[None, :] + 7, 0, 14)
            dc = np.clip(kc[:, None] - qcolv[None, :] + 15, 0, 30)
            vals = rpb[:, dr, dc]
            out[qi, :, :, j, :] = np.where(inwin[None], vals, np.float32(NEG))
    return out.reshape(NLT, 8, 128, 768)


_PROG = {}


def _get_prog(kind, last, lam_init=None):
    key = (kind, last)
    if key not in _PROG:
        _PROG[key] = build_mix(last, lam_init) if kind == "mix" else build_moe(last)
    return _PROG[key]


def kernel(x, c, ctx, c_ctx, w_ada, b_ada, g_norm1, g_norm2, w_in, na_rpb,
           da_lam_q1, da_lam_k1, da_lam_q2, da_lam_k2, da_subln_g,
           gm_ln_g, gm_ln_b, gm_w_s, gm_b_s, w_branch, w_out,
           w_router, w_e1, w_e3, w_e2, g_final):
    f = lambda a: np.ascontiguousarray(np.asarray(a, dtype=np.float32))
    x = f(x)[0]; xc = f(ctx)[0]
    cvec = np.empty((16, 128), np.float32)
    cvec[0::2] = f(c).reshape(8, 128)
    cvec[1::2] = f(c_ctx).reshape(8, 128)
    cosT, ssT = _rope_tables()
    ident_c = np.ones((128, CTX), np.float32); zero_c = np.zeros((128, CTX), np.float32)
    ropeF = np.stack([np.concatenate([ident_c, cosT], 1), np.concatenate([zero_c, ssT], 1)])
    partner = _swap_perm()
    perm512 = np.concatenate([b * 64 + partner for b in range(8)])
    depth = w_in.shape[0]
    for i in range(depth):
        last = i == depth - 1
        lam_init = 0.8 - 0.6 * math.exp(-0.3 * i)
        wi = f(w_in[i])
        w_sw = np.ascontiguousarray(np.concatenate([wi[:, 1024:1536][:, perm512], wi[:, 2560:3072][:, perm512]], 1))
        lamv = np.concatenate([f(da_lam_q1[i]), f(da_lam_k1[i]), f(da_lam_q2[i]), f(da_lam_k2[i])]).reshape(1, 256)
        shared = dict(xfull=x, ctx=xc, cvec=cvec, w_ada=f(w_ada[i]), b_ada=f(b_ada[i]).reshape(1, -1), g1=f(g_norm1[i]).reshape(1, -1),
                      g2=f(g_norm2[i]).reshape(1, -1), w_in=wi, w_sw=w_sw, lamv=lamv, subg=f(da_subln_g[i]).reshape(1, -1),
                      ln_g=f(gm_ln_g[i]).reshape(1, -1), ln_b=f(gm_ln_b[i]).reshape(1, -1),
                      wsT=np.ascontiguousarray(f(gm_w_s[i]).transpose(0, 2, 1)), bsT=np.ascontiguousarray(f(gm_b_s[i]).T),
                      w_br=f(w_branch[i]).reshape(1536, D), w_out=f(w_out[i]), w_r=f(w_router[i]), ropeF=ropeF)
        in_maps = []
        rpb = f(na_rpb[i])
        for cc in range(NCORES):
            xh = np.zeros((NHALO * 128, D), np.float32)
            lo_t = cc * TPC - 256; hi_t = cc * TPC + TPC + 256
            a, b = max(lo_t, 0), min(hi_t, L)
            xh[a - lo_t:b - lo_t] = x[a:b]
            rq = np.stack([np.concatenate([cosT[:, cc * TPC:(cc + 1) * TPC], ident_c], 1),
                           np.concatenate([ssT[:, cc * TPC:(cc + 1) * TPC], zero_c], 1)])
            m = dict(shared)
            m.update(xhalo=xh, ropeQ=np.ascontiguousarray(rq), nabias=_na_bias(rpb, cc))
            in_maps.append(m)
        nc = _get_prog("mix", last, lam_init)
        res = run_bass_kernel_spmd(nc, in_maps, core_ids=list(range(NCORES))).results
        x_mid = np.concatenate([r["xmid"][:TPC] for r in res], 0)
        aff = np.concatenate([r["aff"][:TPC] for r in res], 0)
        mod = res[0]["modout"]
        if not last:
            xc_mid = res[0]["xmid"][TPC:TPC + CTX]
            affc = res[0]["aff"][TPC:TPC + CTX]
        NT = NLT + (0 if last else 1)
        in_maps = []
        for cc in range(NCORES):
            xin = np.zeros((NT * 128, D), np.float32)
            ao = np.zeros((NT * 128, NE), np.float32)
            xin[:TPC] = x_mid[cc * TPC:(cc + 1) * TPC]
            ao[:TPC] = aff[cc * TPC:(cc + 1) * TPC]
            if not last:
                xin[TPC:TPC + 32] = xc_mid[cc * 32:(cc + 1) * 32]
                ao[TPC:TPC + 32] = affc[cc * 32:(cc + 1) * 32]
                affc_in = np.ascontiguousarray(affc.reshape(128, 2 * NE))
            else:
                affc_in = np.zeros((128, 2 * NE), np.float32)
            in_maps.append(dict(xin=xin, affall=np.ascontiguousarray(aff.reshape(128, 128 * NE)), affc=affc_in, affown=ao, modin=mod,
                                g2=f(g_norm2[i]).reshape(1, -1), gfin=f(g_final).reshape(1, -1),
                                w1=f(w_e1[i]), w3=f(w_e3[i]), w2=f(w_e2[i])))
        nc = _get_prog("moe", last)
        res = run_bass_kernel_spmd(nc, in_maps, core_ids=list(range(NCORES))).results
        x = np.ascontiguousarray(np.concatenate([r["xout"][:TPC] for r in res], 0))
        if not last:
            xc = np.ascontiguousarray(np.concatenate([r["xout"][TPC:TPC + 32] for r in res], 0))
    return x.reshape(1, L, D).astype(np.float32)
```
